# Optimizing a Trainium2 kernel written in Bass

```python
import jax, jax.numpy as jnp
from jax import lax
import numpy as np

D_MODEL = 1024
BATCH = 4
SEQ = 4096
DEPTH = 2

N_MIXERS = 2
HEAD_DIM = 64
MEM_LEN = 256
MEM_HEADS = 4
MEM_WIDTH = MEM_HEADS * HEAD_DIM
MIX_WIDTH = D_MODEL - MEM_WIDTH
CONV_WIDTH = 31
MOBA_HEADS = MIX_WIDTH // HEAD_DIM
MOBA_BLOCK = 256
MOBA_TOPK = 3
Q_CHUNK = 64
N_GROUPS = 4
EXPERTS_PER_GROUP = 8
EXPERT_TOPK = 2
D_EXPERT = 512
LN_EPS = 1e-5
DEEPNORM_ALPHA = (2 * DEPTH) ** 0.25
DEEPNORM_BETA = (8 * DEPTH) ** -0.25
N_CONV_LAYERS = (DEPTH + 1) // 2
N_MOBA_LAYERS = DEPTH // 2

kernel_name = "hybrid_conformer_moba_memxattn_hmoe_deepnorm"


def layer_norm(x, g, b):
    xf = x.astype(jnp.float32)
    mu = jnp.mean(xf, axis=-1, keepdims=True)
    var = jnp.mean(jnp.square(xf - mu), axis=-1, keepdims=True)
    return ((xf - mu) * lax.rsqrt(var + LN_EPS) * g + b).astype(x.dtype)


def conformer_conv(u, dw_w, dw_b, ln_g, ln_b):
    a, gate = jnp.split(u, 2, axis=-1)
    h = a * jax.nn.sigmoid(gate)
    h = lax.conv_general_dilated(
        h, dw_w[:, None, :], window_strides=(1,),
        padding=[(CONV_WIDTH - 1, 0)],
        dimension_numbers=('NWC', 'WIO', 'NWC'),
        feature_group_count=MIX_WIDTH) + dw_b
    h = layer_norm(h, ln_g, ln_b)
    return jax.nn.silu(h)


def memory_attention(q, mem_k, mem_v):
    s = jnp.einsum('bshd,bmhd->bhsm', q, mem_k).astype(jnp.float32) * (HEAD_DIM ** -0.5)
    p = jax.nn.softmax(s, axis=-1).astype(q.dtype)
    return jnp.einsum('bhsm,bmhd->bshd', p, mem_v)


def moba_attention(q, k, v):
    b_, h_, s_, hd = q.shape
    n_blocks = -(-s_ // MOBA_BLOCK)
    s_pad = n_blocks * MOBA_BLOCK
    pad = ((0, 0), (0, 0), (0, s_pad - s_), (0, 0))
    q, k, v = jnp.pad(q, pad), jnp.pad(k, pad), jnp.pad(v, pad)
    k_blk = k.reshape(b_, h_, n_blocks, MOBA_BLOCK, hd)
    v_blk = v.reshape(b_, h_, n_blocks, MOBA_BLOCK, hd)
    k_mean = jnp.mean(k_blk.astype(jnp.float32), axis=3)
    n_sel = min(MOBA_TOPK, n_blocks - 1)
    scale = hd ** -0.5
    b_idx = jnp.arange(b_)[:, None, None, None]
    h_idx = jnp.arange(h_)[None, :, None, None]

    def chunk(ci):
        start = ci * Q_CHUNK
        q_c = lax.dynamic_slice_in_dim(q, start, Q_CHUNK, axis=2)
        own = start // MOBA_BLOCK
        q_pos = start + jnp.arange(Q_CHUNK)
        k_own = lax.dynamic_index_in_dim(k_blk, own, axis=2, keepdims=False)
        v_own = lax.dynamic_index_in_dim(v_blk, own, axis=2, keepdims=False)
        k_pos = own * MOBA_BLOCK + jnp.arange(MOBA_BLOCK)
        s_own = jnp.einsum('bhqd,bhkd->bhqk', q_c, k_own).astype(jnp.float32) * scale
        s_own = jnp.where(k_pos[None, :] <= q_pos[:, None], s_own, -jnp.inf)
        if n_sel == 0:
            p = jax.nn.softmax(s_own, axis=-1).astype(v.dtype)
            return jnp.einsum('bhqk,bhkd->bhqd', p, v_own)
        gate = jnp.einsum('bhqd,bhnd->bhqn', q_c.astype(jnp.float32), k_mean)
        gate = jnp.where(jnp.arange(n_blocks) < own, gate, -jnp.inf)
        _, idx = lax.top_k(gate, n_sel)
        valid = idx < own
        k_sel = k_blk[b_idx, h_idx, idx]
        v_sel = v_blk[b_idx, h_idx, idx]
        s_sel = jnp.einsum('bhqd,bhqnkd->bhqnk', q_c, k_sel).astype(jnp.float32) * scale
        s_sel = jnp.where(valid[..., None], s_sel, -jnp.inf)
        s_sel = s_sel.reshape(b_, h_, Q_CHUNK, n_sel * MOBA_BLOCK)
        p = jax.nn.softmax(jnp.concatenate([s_own, s_sel], axis=-1), axis=-1).astype(v.dtype)
        p_own = p[..., :MOBA_BLOCK]
        p_sel = p[..., MOBA_BLOCK:].reshape(b_, h_, Q_CHUNK, n_sel, MOBA_BLOCK)
        return (jnp.einsum('bhqk,bhkd->bhqd', p_own, v_own)
                + jnp.einsum('bhqnk,bhqnkd->bhqd', p_sel, v_sel))

    outs = lax.map(chunk, jnp.arange(s_pad // Q_CHUNK))
    out = jnp.moveaxis(outs, 0, 2).reshape(b_, h_, s_pad, hd)
    return out[:, :, :s_]


def hierarchical_moe(x, w_rg, b_rg, w_re, b_re, w_gate, w_up, w_down):
    b_, s_, d_ = x.shape
    t = x.reshape(-1, d_)
    n_tok = t.shape[0]
    rows = jnp.arange(n_tok)
    g_logits = jnp.dot(t, w_rg).astype(jnp.float32) + b_rg
    g_prob = jax.nn.softmax(g_logits, axis=-1)
    g_idx = jnp.argmax(g_logits, axis=-1)
    g_w = g_prob[rows, g_idx]
    e_logits = (jnp.dot(t, w_re).astype(jnp.float32) + b_re).reshape(n_tok, N_GROUPS, EXPERTS_PER_GROUP)
    e_sel = e_logits[rows, g_idx]
    top_v, top_i = lax.top_k(e_sel, EXPERT_TOPK)
    top_w = jax.nn.softmax(top_v, axis=-1) * g_w[:, None]
    within = jnp.sum(jax.nn.one_hot(top_i, EXPERTS_PER_GROUP, dtype=jnp.float32) * top_w[..., None], axis=1)
    combine = (jax.nn.one_hot(g_idx, N_GROUPS, dtype=jnp.float32)[:, :, None]
               * within[:, None, :]).astype(t.dtype)
    out = jnp.zeros_like(t)
    for g in range(N_GROUPS):
        h = (jax.nn.silu(jnp.einsum('td,edf->tef', t, w_gate[g]))
             * jnp.einsum('td,edf->tef', t, w_up[g]))
        out = out + jnp.einsum('tef,efd->td', h * combine[:, g, :, None], w_down[g])
    return out.reshape(b_, s_, d_)


def setup_inputs(seed: int = 0) -> dict:
    key = jax.random.key(seed)
    ks = jax.random.split(key, 24)
    f32 = jnp.float32
    nrm = lambda k, shape, scale: jax.random.normal(k, shape, f32) * scale
    na, nb, L = N_CONV_LAYERS, N_MOBA_LAYERS, DEPTH
    G, E, F, D = N_GROUPS, EXPERTS_PER_GROUP, D_EXPERT, D_MODEL
    return {
        "x": nrm(ks[0], (BATCH, SEQ, D), 1.0),
        "mem": nrm(ks[1], (BATCH, MEM_LEN, D), 1.0),
        "w_mem_kv": nrm(ks[2], (D, 2 * MEM_WIDTH), D ** -0.5),
        "conv_w_in": nrm(ks[3], (na, D, 2 * MIX_WIDTH + MEM_WIDTH), D ** -0.5),
        "conv_dw_w": nrm(ks[4], (na, CONV_WIDTH, MIX_WIDTH), CONV_WIDTH ** -0.5),
        "conv_dw_b": nrm(ks[5], (na, MIX_WIDTH), 0.02),
        "conv_ln_g": 1.0 + nrm(ks[6], (na, MIX_WIDTH), 0.02),
        "conv_ln_b": nrm(ks[7], (na, MIX_WIDTH), 0.02),
        "moba_w_in": nrm(ks[8], (nb, D, 3 * MIX_WIDTH + MEM_WIDTH), D ** -0.5),
        "w_o": nrm(ks[9], (L, D, D), D ** -0.5 * DEEPNORM_BETA),
        "ln1_g": 1.0 + nrm(ks[10], (L, D), 0.02),
        "ln1_b": nrm(ks[11], (L, D), 0.02),
        "w_rg": nrm(ks[12], (L, D, G), D ** -0.5),
        "b_rg": nrm(ks[13], (L, G), 0.01),
        "w_re": nrm(ks[14], (L, D, G * E), D ** -0.5),
        "b_re": nrm(ks[15], (L, G * E), 0.01),
        "w_gate": nrm(ks[16], (L, G, E, D, F), D ** -0.5),
        "w_up": nrm(ks[17], (L, G, E, D, F), D ** -0.5),
        "w_down": nrm(ks[18], (L, G, E, F, D), F ** -0.5 * DEEPNORM_BETA),
        "ln2_g": 1.0 + nrm(ks[19], (L, D), 0.02),
        "ln2_b": nrm(ks[20], (L, D), 0.02),
    }


def reference(x, mem, w_mem_kv, conv_w_in, conv_dw_w, conv_dw_b, conv_ln_g, conv_ln_b,
              moba_w_in, w_o, ln1_g, ln1_b, w_rg, b_rg, w_re, b_re, w_gate, w_up, w_down,
              ln2_g, ln2_b):
    b_, s_, _ = x.shape
    mem_kv = jnp.dot(mem, w_mem_kv).reshape(b_, MEM_LEN, 2, MEM_HEADS, HEAD_DIM)
    mem_k, mem_v = mem_kv[:, :, 0], mem_kv[:, :, 1]
    for i in range(DEPTH):
        j = i // N_MIXERS
        if i % N_MIXERS == 0:
            u = jnp.dot(x, conv_w_in[j])
            y_mix = conformer_conv(u[..., :2 * MIX_WIDTH], conv_dw_w[j], conv_dw_b[j],
                                   conv_ln_g[j], conv_ln_b[j])
            q_mem = u[..., 2 * MIX_WIDTH:]
        else:
            u = jnp.dot(x, moba_w_in[j])
            qkv = u[..., :3 * MIX_WIDTH].reshape(b_, s_, 3, MOBA_HEADS, HEAD_DIM)
            qkv = jnp.transpose(qkv, (2, 0, 3, 1, 4))
            y_mix = moba_attention(qkv[0], qkv[1], qkv[2])
            y_mix = jnp.transpose(y_mix, (0, 2, 1, 3)).reshape(b_, s_, MIX_WIDTH)
            q_mem = u[..., 3 * MIX_WIDTH:]
        y_mem = memory_attention(q_mem.reshape(b_, s_, MEM_HEADS, HEAD_DIM), mem_k, mem_v)
        y_mem = y_mem.reshape(b_, s_, MEM_WIDTH)
        y = jnp.dot(jnp.concatenate([y_mix, y_mem], axis=-1), w_o[i])
        x = layer_norm(DEEPNORM_ALPHA * x + y, ln1_g[i], ln1_b[i])
        f = hierarchical_moe(x, w_rg[i], b_rg[i], w_re[i], b_re[i], w_gate[i], w_up[i], w_down[i])
        x = layer_norm(DEEPNORM_ALPHA * x + f, ln2_g[i], ln2_b[i])
    return x
```

```python
import contextlib
import numpy as np
import concourse.bass as bass
import concourse.mybir as mybir
from concourse.bass_utils import run_bass_kernel_spmd

F32 = mybir.dt.float32
BF16 = mybir.dt.bfloat16
AF = mybir.ActivationFunctionType
ALU = mybir.AluOpType
AX = mybir.AxisListType

PE, ACT, DVE, POOL, SP = "tensor", "scalar", "vector", "gpsimd", "sync"
ENGINES = [PE, ACT, DVE, POOL, SP]
NRING = 6

D = 1024
HD = 64
MEM = 256
MEMW = 256
MIX = 768
CONVW = 31
BLK = 256
F = 512
LN_EPS = 1e-5
ALPHA = 4.0 ** 0.25
NEG = -30000.0
STAGE = 99
SKIP = ""
BIG = 1.0e30


class Op:
    __slots__ = ("eng", "fn", "is_dma", "waits", "signal", "sigval", "ring", "ringval")

    def __init__(self, eng, fn, is_dma):
        self.eng = eng
        self.fn = fn
        self.is_dma = is_dma
        self.waits = []
        self.signal = False
        self.sigval = None
        self.ring = None
        self.ringval = None


class Sched:
    def __init__(self):
        self.ops = {e: [] for e in ENGINES}
        self.last_w = {}
        self.readers = {}
        self.ring_n = {e: 0 for e in ENGINES}

    def add(self, eng, fn, reads=(), writes=(), dma=False):
        op = Op(eng, fn, dma)
        deps = []
        for k in reads:
            w = self.last_w.get(k)
            if w is not None:
                deps.append(w)
        for k in writes:
            w = self.last_w.get(k)
            if w is not None:
                deps.append(w)
            r = self.readers.get(k)
            if r is not None:
                deps.extend(r[0].values())
                deps.extend(r[1])
        seen = set()
        for d in deps:
            if id(d) in seen or d is op:
                continue
            seen.add(id(d))
            if not d.is_dma:
                if d.eng == eng and eng == PE:
                    continue
                d.signal = True
            op.waits.append(d)
        for k in reads:
            r = self.readers.get(k)
            if r is None:
                r = self.readers[k] = ({}, [])
            if dma:
                r[1].append(op)
            else:
                r[0][eng] = op
        for k in writes:
            self.last_w[k] = op
            self.readers[k] = ({}, [])
        if dma:
            i = self.ring_n[eng]
            self.ring_n[eng] += 1
            op.ring = (eng, i % NRING)
            op.ringval = 16 * (i // NRING + 1)
        self.ops[eng].append(op)
        return op

    def emit(self, nc):
        with contextlib.ExitStack() as es:
            esem = {e: es.enter_context(nc.semaphore("s_" + e)) for e in ENGINES}
            rsem = {}
            for e in ENGINES:
                for i in range(min(NRING, self.ring_n[e])):
                    rsem[(e, i)] = es.enter_context(nc.semaphore("r_%s_%d" % (e, i)))
            for e in ENGINES:
                c = 0
                for op in self.ops[e]:
                    if (not op.is_dma) and op.signal:
                        c += 1
                        op.sigval = c
            block = es.enter_context(nc.Block())

            def make(e):
                def body(eng):
                    waited = {}
                    ring_last = {}
                    for op in self.ops[e]:
                        ws = []
                        for d in op.waits:
                            if d.is_dma:
                                ws.append((rsem[d.ring], d.ringval, d.ring))
                            else:
                                ws.append((esem[d.eng], d.sigval, d.eng))
                        if op.is_dma:
                            prev = ring_last.get(op.ring)
                            if prev is not None:
                                ws.append((rsem[op.ring], prev, op.ring))
                            ring_last[op.ring] = op.ringval
                        best = {}
                        for s, v, key in ws:
                            if waited.get(key, 0) >= v:
                                continue
                            if best.get(key, (None, 0))[1] < v:
                                best[key] = (s, v)
                        for key, (s, v) in best.items():
                            eng.wait_ge(s, v)
                            waited[key] = v
                        ins = op.fn(eng)
                        if op.is_dma:
                            ins.then_inc(rsem[op.ring], 16)
                        elif op.signal:
                            ins.then_inc(esem[e], 1)
                    for rk, v in ring_last.items():
                        if waited.get(rk, 0) < v:
                            eng.wait_ge(rsem[rk], v)
                return body

            for e in ENGINES:
                if self.ops[e]:
                    getattr(block, e)(make(e))


class B:
    def __init__(self, kind, T, NE, G):
        self.kind = kind
        self.T = T
        self.NT = T // 128
        self.NG = T // 512
        self.NE = NE
        self.G = G
        self.EPG = NE // G
        self.NR = G + NE
        self.nc = bass.Bass("TRN2", target_bir_lowering=False)
        self.S = Sched()
        self.es = contextlib.ExitStack()
        self.psrot = {}

    def din(self, name, shape, dt=F32):
        return self.nc.dram_tensor(name, list(shape), dt, kind="ExternalInput").ap()

    def sb(self, name, shape, dt):
        return self.es.enter_context(self.nc.sbuf_tensor(name, list(shape), dt))

    def mm(self, out, lhsT, rhs, start, stop, reads, writes):
        self.S.add(PE, lambda e: e.matmul(out, lhsT, rhs, start=start, stop=stop), reads, writes)

    def tr(self, out, in_, ident, reads, writes):
        self.S.add(PE, lambda e: e.transpose(out, in_, ident), reads, writes)

    def act(self, out, in_, func, reads, writes, bias=None, scale=None):
        kw = {}
        if bias is not None:
            kw["bias"] = bias
        if scale is not None:
            kw["scale"] = scale
        self.S.add(ACT, lambda e: e.activation(out=out, in_=in_, func=func, **kw), reads, writes)

    def tt(self, eng, out, in0, in1, op, reads, writes):
        self.S.add(eng, lambda e: e.tensor_tensor(out=out, in0=in0, in1=in1, op=op), reads, writes)

    def ts(self, eng, out, in0, s1, s2, op0, op1, reads, writes):
        if op1 is None:
            self.S.add(eng, lambda e: e.tensor_scalar(out=out, in0=in0, scalar1=s1, scalar2=None, op0=op0), reads, writes)
        else:
            self.S.add(eng, lambda e: e.tensor_scalar(out=out, in0=in0, scalar1=s1, scalar2=s2, op0=op0, op1=op1), reads, writes)

    def stt(self, eng, out, in0, scalar, in1, op0, op1, reads, writes):
        self.S.add(eng, lambda e: e.scalar_tensor_tensor(out=out, in0=in0, scalar=scalar, in1=in1, op0=op0, op1=op1), reads, writes)

    def cp(self, eng, out, in_, reads, writes):
        if eng == ACT:
            self.S.add(ACT, lambda e: e.copy(out=out, in_=in_), reads, writes)
        else:
            self.S.add(eng, lambda e: e.tensor_copy(out=out, in_=in_), reads, writes)

    def rsqrt_eps(self, ap, keys):
        self.ts(DVE, ap, ap, LN_EPS, None, ALU.add, None, keys, keys)
        self.act(ap, ap, AF.Sqrt, keys, keys)
        self.S.add(DVE, lambda e: e.reciprocal(out=ap, in_=ap), keys, keys)

    def memset(self, eng, ap, val, writes):
        self.S.add(eng, lambda e: e.memset(ap, val), (), writes)

    def dma(self, eng, out, in_, reads, writes):
        self.S.add(eng, lambda e: e.dma_start(out=out, in_=in_), reads, writes, dma=True)

    def psb(self, grp, banks):
        i = self.psrot.get(grp, 0)
        self.psrot[grp] = i + 1
        b = banks[i % len(banks)]
        return self.ps[b], ("ps", b)

    def setup_common(self):
        nc, T, NT = self.nc, self.T, self.NT
        self.ps = [self.es.enter_context(nc.psum_tensor("ps%d" % i, [128, 512], F32)) for i in range(8)]
        self.R = self.sb("R", [128, NT, D], F32)
        self.XT = self.sb("XT", [128, 8, T], BF16)
        self.WB = self.sb("WB", [128, 6, 4096], BF16)
        self.identf = self.sb("identf", [128, 128], F32)
        self.identb = self.sb("identb", [128, 128], BF16)
        self.onesb = self.sb("onesb", [128, 128], BF16)
        self.onesf = self.sb("onesf", [128, 128], F32)
        self.lnp = self.sb("lnpb", [128, 2, D], F32)
        self.SCR = self.sb("SCR", [128, 23616], BF16)
        self.scr_off = 0
        self.comb = self.sb("comb", [128, NT, self.NE], F32)
        self.memKT = self.sb("memKT", [128, 2, MEM], BF16)
        self.memV = self.sb("memV", [128, 2, MEMW], BF16)
        self.qmT = self.XT[:, 6:8, :]
        self.brb = self.sb("brb", [128, self.NR], F32)
        self.Wr = self.sb("Wr", [128, 8, self.NR], F32)
        self.small = self.sb("small", [128, 4, 64], F32)
        self.stats = self.sb("stats", [128, 2, 2, 6], F32)
        self.mv = self.sb("mv", [128, 2, 2], F32)
        self.d_xtok = self.din("xtok", [T, D])
        self.d_ident = self.din("ident", [128, 128])
        self.d_memT = self.din("memT", [D, MEM])
        self.d_wmem = self.din("w_mem_kv", [D, 2 * MEMW])
        self.d_wo = self.din("w_o", [D, D])
        self.d_ln = self.din("lnp", [4, D])
        self.d_wr = self.din("w_r", [D, self.NR])
        self.d_br = self.din("b_r", [1, self.NR])
        self.d_wg = self.din("w_gate", [self.NE, D, F])
        self.d_wu = self.din("w_up", [self.NE, D, F])
        self.d_wd = self.din("w_down", [self.NE, F, D])
        self.d_out = nc.dram_tensor("out", [T, D], F32, kind="ExternalOutput").ap()

        self.dma(SP, self.R[:], self.d_xtok.rearrange("(n p) d -> p n d", p=128), (), [("R", i, h) for i in range(NT) for h in range(2)])
        self.dma(SP, self.identf[:], self.d_ident, (), ["identf"])
        self.dma(POOL, self.identb[:], self.d_ident, (), ["identb"])
        self.memset(DVE, self.onesb[:], 1.0, ["onesb"])
        self.memset(DVE, self.onesf[:], 1.0 / MIX, ["onesf"])
        self.load_lnp(0)
        self.dma(SP, self.brb[:], self.d_br[0, :].partition_broadcast(128), (), ["brb"])
        self.dma(SP, self.Wr[:], self.d_wr.rearrange("(k p) r -> p k r", p=128), (), ["Wr"])

    def load_lnp(self, which):
        for i in range(2):
            self.dma(SP, self.lnp[:, i, :], self.d_ln[2 * which + i, :].partition_broadcast(128), (), [("lnp", i)])

    def carve(self, shape, dt):
        n = 1
        for d_ in shape[1:]:
            n *= d_
        nb = n * (2 if dt == F32 else 1)
        nb = (nb + 15) // 16 * 16
        off = self.scr_off
        self.scr_off += nb
        assert self.scr_off <= 23616, ("scratch arena overflow", self.scr_off)
        ap = self.SCR[0:shape[0], off:off + n * (2 if dt == F32 else 1)]
        if dt == F32:
            ap = ap.bitcast(F32)
        if len(shape) == 3:
            ap = ap.rearrange("p (a b) -> p a b", a=shape[1])
        elif len(shape) == 4:
            ap = ap.rearrange("p (a b c) -> p a b c", a=shape[1], b=shape[2])
        return ap

    def mem_kv(self):
        memTb = self.WB[:, 4, 0:8 * MEM].rearrange("p (k m) -> p k m", k=8)
        Wm = self.WB[:, 5, 0:8 * 512].rearrange("p (k m) -> p k m", k=8)
        self.dma(POOL, memTb, self.d_memT.rearrange("(k p) m -> p k m", p=128), (), [("WB", 4)])
        self.dma(POOL, Wm, self.d_wmem.rearrange("(k p) m -> p k m", p=128), (), [("WB", 5)])
        for c in range(2):
            ps, pk = self.psb("a", [0, 1])
            for dk in range(8):
                self.mm(ps[:, 0:MEM], Wm[:, dk, c * 128:(c + 1) * 128], memTb[:, dk, :], dk == 0, dk == 7,
                        [("WB", 4), ("WB", 5)], [pk])
            self.cp(DVE, self.memKT[:, c, :], ps[:, 0:MEM], [pk], [("memKT", c)])
        for mc in range(2):
            ps, pk = self.psb("a", [0, 1])
            for dk in range(8):
                self.mm(ps[:, 0:MEMW], memTb[:, dk, mc * 128:(mc + 1) * 128], Wm[:, dk, MEMW:2 * MEMW], dk == 0, dk == 7,
                        [("WB", 4), ("WB", 5)], [pk])
            self.cp(DVE, self.memV[:, mc, :], ps[:, 0:MEMW], [pk], [("memV", mc)])

    def mem_attention(self, c0, ncols, ydst, ykeys):
        PT = self.PTm
        for h in range(4):
            c, poff = h // 2, 64 * (h % 2)
            for mc in range(2):
                ps, pk = self.psb("ms", [0, 1])
                self.mm(ps[:, 0:ncols], self.memKT[poff:poff + 64, c, mc * 128:(mc + 1) * 128],
                        self.qmT[poff:poff + 64, c, c0:c0 + ncols], True, True,
                        [("memKT", c)] + [("XT", 6 + c, c0 // 128 + t_) for t_ in range(ncols // 128)], [pk])
                self.act(PT[:, mc, 0:ncols], ps[:, 0:ncols], AF.Exp, [pk], [("PTm", mc)], scale=HD ** -0.5)
            pso, pok = self.psb("mo", [6])
            psz, pzk = self.psb("mz", [7])
            for mc in range(2):
                self.mm(pso[:, 0:ncols], self.memV[:, mc, c * 128:(c + 1) * 128], PT[:, mc, 0:ncols], mc == 0, mc == 1,
                        [("memV", mc), ("PTm", mc)], [pok])
            for mc in range(2):
                self.mm(psz[:, 0:ncols], self.onesb[:, :], PT[:, mc, 0:ncols], mc == 0, mc == 1,
                        ["onesb", ("PTm", mc)], [pzk])
            rz = self.rzm
            self.S.add(DVE, lambda e, o=rz[poff:poff + 64, 0:ncols], i=psz[poff:poff + 64, 0:ncols]: e.reciprocal(out=o, in_=i),
                       [pzk], ["rzm"])
            self.tt(DVE, ydst(c, poff), pso[poff:poff + 64, 0:ncols], rz[poff:poff + 64, 0:ncols], ALU.mult,
                    [pok, "rzm"], ykeys(c))

    def ln_tile(self, i, which):
        R = self.R
        st = self.stats[:, i % 2]
        mv = self.mv[:, i % 2]
        rk = [("R", i, 0), ("R", i, 1)]
        for h in range(2):
            self.S.add(DVE, lambda e, o=st[:, h, :], a=R[:, i, h * 512:(h + 1) * 512]: e.bn_stats(out=o, in_=a), rk, [("stats", i % 2, h)])
        self.S.add(DVE, lambda e, o=mv[:, :], a=st[:, :, :]: e.bn_aggr(out=o, in_=a), [("stats", i % 2, 0), ("stats", i % 2, 1)], [("mv", i % 2)])
        self.rsqrt_eps(mv[:, 1:2], [("mv", i % 2)])
        self.ts(DVE, R[:, i, :], R[:, i, :], mv[:, 0:1], mv[:, 1:2], ALU.subtract, ALU.mult, rk + [("mv", i % 2)], rk)
        self.tt(POOL, R[:, i, :], R[:, i, :], self.lnp[:, 0, :], ALU.mult, rk + [("lnp", 0)], rk)
        self.tt(POOL, R[:, i, :], R[:, i, :], self.lnp[:, 1, :], ALU.add, rk + [("lnp", 1)], rk)

    def transpose_tile(self, i, route):
        R, XT = self.R, self.XT
        rk = [("R", i, 0), ("R", i, 1)]
        pa, pak = self.psb("tp", [4, 6])
        pb = self.ps[5] if pa is self.ps[4] else self.ps[7]
        pbk = ("ps", 5) if pa is self.ps[4] else ("ps", 7)
        for dk in range(8):
            p, k = (pa, pak) if dk < 4 else (pb, pbk)
            self.tr(p[:, (dk % 4) * 128:(dk % 4 + 1) * 128], R[:, i, dk * 128:(dk + 1) * 128], self.identf[:, :], rk + ["identf"], [k])
        xtk = [("XT", dk, i) for dk in range(8)]
        self.cp(ACT, XT[:, 0:4, i * 128:(i + 1) * 128], pa[:, :].rearrange("p (k t) -> p k t", k=4), [pak], xtk[0:4])
        self.cp(ACT, XT[:, 4:8, i * 128:(i + 1) * 128], pb[:, :].rearrange("p (k t) -> p k t", k=4), [pbk], xtk[4:8])
        if route:
            xf = self.xTf[:, i % 2]
            self.cp(DVE, xf[:, 0:4, :], pa[:, :].rearrange("p (k t) -> p k t", k=4), [pak], [("xTf", i % 2, 0)])
            self.cp(DVE, xf[:, 4:8, :], pb[:, :].rearrange("p (k t) -> p k t", k=4), [pbk], [("xTf", i % 2, 1)])
            self.routing(i, xf)

    def routing(self, i, xf):
        G, NE, EPG, NR = self.G, self.NE, self.EPG, self.NR
        pl, plk = self.psb("rt", [2, 3])
        for dk in range(8):
            self.mm(pl[:, 0:NR], xf[:, dk, :], self.Wr[:, dk, :], dk == 0, dk == 7,
                    [("xTf", i % 2, 0), ("xTf", i % 2, 1), "Wr"], [plk])
        sm = self.small[:, i % 2]
        sk = ("small", i % 2)
        lg = sm[:, 0:NR]
        self.tt(DVE, lg, pl[:, 0:NR], self.brb[:, :], ALU.add, [plk, "brb"], [sk])
        gmax = sm[:, 40:41]
        self.S.add(DVE, lambda e: e.tensor_reduce(out=gmax, in_=sm[:, 0:G], axis=AX.X, op=ALU.max), [sk], [sk])
        ohg = sm[:, 41:41 + G]
        self.ts(DVE, ohg, sm[:, 0:G], gmax, None, ALU.is_equal, None, [sk], [sk])
        ex = sm[:, 48:48 + G]
        self.ts(DVE, ex, sm[:, 0:G], gmax, None, ALU.subtract, None, [sk], [sk])
        self.act(ex, ex, AF.Exp, [sk], [sk])
        gs = sm[:, 52:53]
        self.S.add(DVE, lambda e: e.tensor_reduce(out=gs, in_=ex, axis=AX.X, op=ALU.add), [sk], [sk])
        self.S.add(DVE, lambda e: e.reciprocal(out=gs, in_=gs), [sk], [sk])
        pen = sm[:, 53:53 + G]
        self.ts(DVE, pen, ohg, -1.0, BIG, ALU.add, ALU.mult, [sk], [sk])
        sm2 = self.small[:, 2 + i % 2]
        sk2 = ("small", 2 + i % 2)
        m32 = sm2[:, 0:NE]
        for g in range(G):
            self.ts(DVE, m32[:, g * EPG:(g + 1) * EPG], sm[:, G + g * EPG:G + (g + 1) * EPG], pen[:, g:g + 1], None, ALU.add, None,
                    [sk], [sk2])
        top8 = sm2[:, 32:40]
        self.S.add(DVE, lambda e: e.max(out=top8, in_=m32), [sk2], [sk2])
        dd = sm2[:, 40:41]
        self.tt(DVE, dd, top8[:, 1:2], top8[:, 0:1], ALU.subtract, [sk2], [sk2])
        self.act(dd, dd, AF.Exp, [sk2], [sk2])
        self.ts(DVE, dd, dd, 1.0, None, ALU.add, None, [sk2], [sk2])
        w1 = sm2[:, 41:42]
        self.S.add(DVE, lambda e: e.reciprocal(out=w1, in_=dd), [sk2], [sk2])
        w2 = sm2[:, 42:43]
        self.ts(DVE, w2, w1, -1.0, 1.0, ALU.mult, ALU.add, [sk2], [sk2])
        self.tt(DVE, w1, w1, gs, ALU.mult, [sk2, sk], [sk2])
        self.tt(DVE, w2, w2, gs, ALU.mult, [sk2, sk], [sk2])
        cb = self.comb[:, i, :]
        ck = ("comb", i)
        tmp = self.small[:, i % 2, 0:NE]
        self.ts(DVE, cb, m32, top8[:, 0:1], w1, ALU.is_equal, ALU.mult, [sk2], [ck])
        self.ts(DVE, tmp, m32, top8[:, 1:2], w2, ALU.is_equal, ALU.mult, [sk2, sk], [sk])
        self.tt(DVE, cb, cb, tmp, ALU.add, [ck, sk], [ck])

    def moe(self):
        T, NT, NG, NE = self.T, self.NT, self.NG, self.NE
        R, XT, WB = self.R, self.XT, self.WB
        for i in range(NT):
            rk = [("R", i, 0), ("R", i, 1)]
            self.ts(POOL, R[:, i, :], R[:, i, :], ALPHA, None, ALU.mult, None, rk, rk)

        def wviews(slot):
            wg = WB[:, 3 * slot + 0, :].rearrange("p (k f) -> p k f", k=8)
            wu = WB[:, 3 * slot + 1, :].rearrange("p (k f) -> p k f", k=8)
            wd = WB[:, 3 * slot + 2, :].rearrange("p (k f) -> p k f", k=4)
            return wg, wu, wd

        def load(e):
            slot = e % 2
            wg, wu, wd = wviews(slot)
            self.dma(POOL, wg, self.d_wg[e].rearrange("(k p) f -> p k f", p=128), (), [("WB", 3 * slot + 0)])
            self.dma(POOL, wu, self.d_wu[e].rearrange("(k p) f -> p k f", p=128), (), [("WB", 3 * slot + 1)])
            self.dma(POOL, wd, self.d_wd[e].rearrange("(k p) f -> p k f", p=128), (), [("WB", 3 * slot + 2)])

        load(0)
        hcount = 0
        for e in range(NE):
            slot = e % 2
            if e + 1 < NE:
                load(e + 1)
            wg, wu, wd = wviews(slot)
            kg, ku, kd = ("WB", 3 * slot), ("WB", 3 * slot + 1), ("WB", 3 * slot + 2)
            for tg in range(NG):
                hb = hcount % 2
                hcount += 1
                cols = slice(tg * 512, (tg + 1) * 512)
                xk = [("XT", dk, tg * 4 + j) for dk in range(8) for j in range(4)]
                for fc in range(4):
                    pg, pgk = self.psb("mg", [0, 1])
                    pu, puk = self.psb("mu", [2, 3])
                    for dk in range(8):
                        self.mm(pg[:, :], wg[:, dk, fc * 128:(fc + 1) * 128], XT[:, dk, cols], dk == 0, dk == 7, [kg] + xk, [pgk])
                    for dk in range(8):
                        self.mm(pu[:, :], wu[:, dk, fc * 128:(fc + 1) * 128], XT[:, dk, cols], dk == 0, dk == 7, [ku] + xk, [puk])
                    sb = (hcount * 4 + fc) % 2
                    self.act(self.sT[:, sb, :], pg[:, :], AF.Silu, [pgk], [("sT", sb)])
                    self.tt(DVE, self.hT[:, hb, fc, :], pu[:, :], self.sT[:, sb, :], ALU.mult, [puk, ("sT", sb)], [("hT", hb, fc)])
                for tt_ in range(4):
                    i = tg * 4 + tt_
                    for dh in range(2):
                        po, pok = self.psb("mo2", [4, 5, 6, 7])
                        for fc in range(4):
                            self.mm(po[:, :], self.hT[:, hb, fc, tt_ * 128:(tt_ + 1) * 128], wd[:, fc, dh * 512:(dh + 1) * 512],
                                    fc == 0, fc == 3, [kd, ("hT", hb, fc)], [pok])
                        self.stt(DVE, R[:, i, dh * 512:(dh + 1) * 512], po[:, :], self.comb[:, i, e:e + 1], R[:, i, dh * 512:(dh + 1) * 512],
                                 ALU.mult, ALU.add, [pok, ("comb", i), ("R", i, dh)], [("R", i, dh)])

    def wo_ln1(self, tg, Wo, wok, yT, ykeys_all):
        R = self.R
        for tt_ in range(4):
            i = tg * 4 + tt_
            for dh in range(2):
                pw, pwk = self.psb("wo", [2, 3])
                for jk in range(8):
                    self.mm(pw[:, :], yT(jk, tt_), Wo[:, jk, dh * 512:(dh + 1) * 512], jk == 0, jk == 7, [wok] + ykeys_all(tt_), [pwk])
                self.stt(DVE, R[:, i, dh * 512:(dh + 1) * 512], R[:, i, dh * 512:(dh + 1) * 512], ALPHA, pw[:, :], ALU.mult, ALU.add,
                         [pwk, ("R", i, dh)], [("R", i, dh)])
            self.ln_tile(i, 0)

    def conv_mixer(self):
        nc, T, NT, NG = self.nc, self.T, self.NT, self.NG
        WB = self.WB
        d_xTh = self.din("xTh", [D, 32 + T])
        d_win = self.din("w_in", [D, 2 * MIX + MEMW])
        d_cw = self.din("conv_w", [128, 6, CONVW])
        d_cv = self.din("conv_v", [128, 3, 6])
        Win = WB[:, 0:4, :].rearrange("p s f -> p (s f)")[:, 0:8 * 1792].rearrange("p (k f) -> p k f", k=8)
        Wo = WB[:, 4:6, :].rearrange("p s f -> p (s f)").rearrange("p (k f) -> p k f", k=8)
        wink = [("WB", s) for s in range(4)]
        self.scr_off = 0
        cw = self.carve([128, 6, CONVW + 1], F32)[:, :, 0:CONVW]
        cv = self.carve([128, 3, 6], F32)
        xg = self.carve([128, 1, 8, 512], BF16)
        xh = self.carve([128, 8, 32], BF16)
        hT = self.carve([128, 2, 6, 544], BF16)
        acc = self.carve([128, 6, 512], F32)
        sq = self.carve([128, 1, 512], F32)
        sg = self.carve([128, 1, 512], F32)
        mean = self.carve([128, 512], F32)
        rstd = self.carve([128, 512], F32)
        self.PTm = self.carve([128, 2, 512], BF16)
        self.rzm = self.carve([128, 512], F32)
        XT = self.XT

        self.mem_kv()
        self.dma(POOL, Win, d_win.rearrange("(k p) f -> p k f", p=128), (), wink)
        self.dma(POOL, Wo, self.d_wo.rearrange("(k p) f -> p k f", p=128), [("memKT", 0), ("memKT", 1), ("memV", 0), ("memV", 1)], [("WB", 4), ("WB", 5)])
        self.dma(SP, cw, d_cw, (), ["cw"])
        self.dma(SP, cv, d_cv, (), ["cv"])
        self.dma(POOL, xh, d_xTh[:, 0:32].rearrange("(k p) t -> p k t", p=128), (), ["xh"])

        def glu(j, pa, pak, pg, pgk, hdst, hkey, n):
            sb_ = 0
            self.psrot["sgr"] = self.psrot.get("sgr", 0) + 1
            self.act(sg[:, sb_, 0:n], pg[:, 0:n], AF.Sigmoid, [pgk], [("sg", sb_)])
            self.tt(DVE, hdst, pa[:, 0:n], sg[:, sb_, 0:n], ALU.mult, [pak, ("sg", sb_)], [hkey])

        for j in range(6):
            pa, pak = self.psb("ia", [0, 1])
            pg, pgk = self.psb("ig", [2, 3])
            for dk in range(8):
                self.mm(pa[:, 0:32], Win[:, dk, j * 128:(j + 1) * 128], xh[:, dk, :], dk == 0, dk == 7, wink + ["xh"], [pak])
            for dk in range(8):
                self.mm(pg[:, 0:32], Win[:, dk, MIX + j * 128:MIX + (j + 1) * 128], xh[:, dk, :], dk == 0, dk == 7, wink + ["xh"], [pgk])
            glu(j, pa, pak, pg, pgk, hT[:, 0, j, 0:32], ("hTc", 0, j, "halo"), 32)

        for tg in range(NG):
            hb = tg % 2
            xb = 0
            self.dma(POOL, xg[:, xb], d_xTh[:, 32 + tg * 512:32 + (tg + 1) * 512].rearrange("(k p) t -> p k t", p=128), (), [("xg", xb)])
            for j in range(6):
                pa, pak = self.psb("ia", [0, 1])
                pg, pgk = self.psb("ig", [2, 3])
                for dk in range(8):
                    self.mm(pa[:, :], Win[:, dk, j * 128:(j + 1) * 128], xg[:, xb, dk, :], dk == 0, dk == 7, wink + [("xg", xb)], [pak])
                for dk in range(8):
                    self.mm(pg[:, :], Win[:, dk, MIX + j * 128:MIX + (j + 1) * 128], xg[:, xb, dk, :], dk == 0, dk == 7, wink + [("xg", xb)], [pgk])
                glu(j, pa, pak, pg, pgk, hT[:, hb, j, 32:544], ("hTc", hb, j, "main"), 512)
            for c in range(2):
                pq, pqk = self.psb("ia", [0, 1])
                for dk in range(8):
                    self.mm(pq[:, :], Win[:, dk, 2 * MIX + c * 128:2 * MIX + (c + 1) * 128], xg[:, xb, dk, :], dk == 0, dk == 7, wink + [("xg", xb)], [pqk])
                self.cp(ACT, self.qmT[:, c, tg * 512:(tg + 1) * 512], pq[:, :], [pqk], [("XT", 6 + c, tg * 4 + t_) for t_ in range(4)])
            for j in range(6):
                eng = DVE
                hk = [("hTc", hb, j, "halo"), ("hTc", hb, j, "main")]
                ak = ("acc", j)
                self.ts(eng, acc[:, j, :], hT[:, hb, j, 2:514], cw[:, j, 0:1], cv[:, 0, j:j + 1], ALU.mult, ALU.add, hk + ["cw", "cv"], [ak])
                for k in range(1, CONVW):
                    self.stt(eng, acc[:, j, :], hT[:, hb, j, 2 + k:514 + k], cw[:, j, k:k + 1], acc[:, j, :], ALU.mult, ALU.add, hk + ["cw", ak], [ak])
                if tg + 1 < NG:
                    self.cp(eng, hT[:, 1 - hb, j, 0:32], hT[:, hb, j, 512:544], hk, [("hTc", 1 - hb, j, "halo")])
            pm, pmk = self.psb("st", [4])
            pq2, pq2k = self.psb("st2", [5])
            for j in range(6):
                self.mm(pm[:, :], self.onesf[:, :], acc[:, j, :], j == 0, j == 5, ["onesf", ("acc", j)], [pmk])
            for j in range(6):
                sb_ = 0
                self.act(sq[:, sb_, :], acc[:, j, :], AF.Square, [("acc", j)], [("sq", sb_)])
                self.mm(pq2[:, :], self.onesf[:, :], sq[:, sb_, :], j == 0, j == 5, ["onesf", ("sq", sb_)], [pq2k])
            self.cp(DVE, mean[:, :], pm[:, :], [pmk], ["mean"])
            self.tt(DVE, rstd[:, :], mean[:, :], mean[:, :], ALU.mult, ["mean"], ["rstd"])
            self.tt(DVE, rstd[:, :], pq2[:, :], rstd[:, :], ALU.subtract, [pq2k, "rstd"], ["rstd"])
            self.rsqrt_eps(rstd[:, :], ["rstd"])
            for j in range(6):
                eng = DVE if j % 2 == 0 else POOL
                ak = ("acc", j)
                self.tt(eng, acc[:, j, :], acc[:, j, :], mean[:, :], ALU.subtract, [ak, "mean"], [ak])
                self.tt(eng, acc[:, j, :], acc[:, j, :], rstd[:, :], ALU.mult, [ak, "rstd"], [ak])
                self.act(XT[:, j, tg * 512:(tg + 1) * 512], acc[:, j, :], AF.Silu, [ak, "cv"], [("XT", j, tg * 4 + t_) for t_ in range(4)],
                         bias=cv[:, 2, j:j + 1], scale=cv[:, 1, j:j + 1])
            self.mem_attention(tg * 512, 512, lambda c, poff, tg=tg: XT[poff:poff + 64, 6 + c, tg * 512:(tg + 1) * 512],
                               lambda c, tg=tg: [("XT", 6 + c, tg * 4 + j_) for j_ in range(4)])
            self.wo_ln1(tg, Wo, ("WB", 4), lambda jk, tt_, tg=tg: XT[:, jk, (tg * 4 + tt_) * 128:(tg * 4 + tt_ + 1) * 128],
                        lambda tt_, tg=tg: [("XT", jk, tg * 4 + tt_) for jk in range(8)] + [("WB", 5)])


    def moba_mixer(self):
        nc, T, NT, NG = self.nc, self.T, self.NT, self.NG
        WB, XT = self.WB, self.XT
        NBo = T // BLK
        NB2 = 2 * NBo
        NSG = 2 * T // 512
        NH = MIX // HD
        WIN = 3 * MIX + MEMW
        d_xsT = self.din("xsT", [D, 2 * T])
        d_win = self.din("w_in", [D, WIN])
        d_padb = self.din("padb", [1, 16])
        d_eind = self.din("eind", [16, 2 * T])
        d_cm = self.din("cmask", [128, 2, BLK])
        kT_d = nc.dram_tensor("kT_d", [NH, HD, 2 * T], BF16, kind="Internal").ap()
        qT_d = nc.dram_tensor("qT_d", [NH, HD, T], BF16, kind="Internal").ap()
        v_d = nc.dram_tensor("v_d", [2 * T, MIX], BF16, kind="Internal").ap()
        Win = WB[:, 0:5, :].rearrange("p s f -> p (s f)").rearrange("p (k f) -> p k f", k=8)
        wink = [("WB", s_) for s_ in range(5)]
        Wo = WB[:, 0:2, :].rearrange("p s f -> p (s f)").rearrange("p (k f) -> p k f", k=8)
        self.scr_off = 0
        xg = self.carve([128, 2, 8, 512], BF16)
        kst = self.carve([64, 4, 512], BF16)
        vst = self.carve([128, 2, MIX], BF16)
        kmT = self.carve([64, NH, 16], F32)
        kmb = self.carve([64, NH, 16], BF16)
        maskb = self.carve([128, NBo, 16], F32)
        padbc = self.carve([128, 16], F32)
        cm = self.carve([128, 2, BLK], BF16)
        bqw = self.carve([128, 2, 2, 128], BF16)
        gm = self.carve([128, 2, 2, 32], F32)
        PT = self.carve([128, 3, BLK], BF16)
        rz = self.carve([128, 2, BLK], F32)
        self.PTm = self.carve([128, 2, 512], BF16)
        self.rzm = self.carve([128, 512], F32)
        KA = WB[:, 2:4, :]
        QA = WB[:, 5, :].rearrange("p (b t) -> p b t", b=2)
        Vh = WB[:, 4, :].rearrange("p (n c) -> p n c", c=128)

        self.mem_kv()
        self.dma(POOL, Win, d_win.rearrange("(k p) f -> p k f", p=128), (), wink)
        self.dma(SP, padbc, d_padb[0, :].partition_broadcast(128), (), ["padbc"])
        self.dma(POOL, cm, d_cm, (), ["cm"])
        self.memset(DVE, kmT, 0.0, ["kmT"])
        self.memset(DVE, bqw, 0.0, [("bqw", 0), ("bqw", 1)])
        self.memset(DVE, maskb, 0.0, ["maskb"])
        for i in range(NBo):
            self.memset(DVE, maskb[:, i, NBo + i:16], -BIG, ["maskb"])
            self.tt(DVE, maskb[:, i, :], maskb[:, i, :], padbc[:, :], ALU.add, ["maskb", "padbc"], ["maskb"])

        cpi = 0
        for sg in range(NSG):
            xb = sg % 2
            own = sg >= NSG // 2
            osg = sg - NSG // 2
            self.dma(POOL, xg[:, xb], d_xsT[:, sg * 512:(sg + 1) * 512].rearrange("(k p) t -> p k t", p=128), (), [("xg", xb)])
            for h in range(NH):
                pk_, pkk = self.psb("ia", [0, 1])
                for dk in range(8):
                    self.mm(pk_[0:64, :], Win[:, dk, MIX + h * HD:MIX + (h + 1) * HD], xg[:, xb, dk, :], dk == 0, dk == 7, wink + [("xg", xb)], [pkk])
                sb_ = cpi % 4
                cpi += 1
                for b2 in range(2):
                    self.S.add(ACT, lambda e, o=kst[:, sb_, b2 * BLK:(b2 + 1) * BLK], a=pk_[0:64, b2 * BLK:(b2 + 1) * BLK],
                               ac=kmT[:, h, sg * 2 + b2:sg * 2 + b2 + 1]: e.activation(out=o, in_=a, func=AF.Copy, accum_out=ac),
                               [pkk, "kmT"], [("kst", sb_), "kmT"])
                if "s" not in SKIP:
                    self.dma(SP, kT_d[h, :, sg * 512:(sg + 1) * 512], kst[:, sb_, :], [("kst", sb_)], [("kTd", sg, h)])
            for tt_ in range(4):
                pv, pvk = self.psb("ig", [2, 3])
                pv2, pv2k = self.psb("iv2", [4, 5])
                for dk in range(8):
                    self.mm(pv[:, :], xg[:, xb, dk, tt_ * 128:(tt_ + 1) * 128], Win[:, dk, 2 * MIX:2 * MIX + 512], dk == 0, dk == 7, wink + [("xg", xb)], [pvk])
                for dk in range(8):
                    self.mm(pv2[:, 0:256], xg[:, xb, dk, tt_ * 128:(tt_ + 1) * 128], Win[:, dk, 2 * MIX + 512:3 * MIX], dk == 0, dk == 7, wink + [("xg", xb)], [pv2k])
                vb_ = tt_ % 2
                self.cp(ACT, vst[:, vb_, 0:512], pv[:, :], [pvk], [("vst", vb_)])
                self.cp(DVE, vst[:, vb_, 512:MIX], pv2[:, 0:256], [pv2k], [("vst", vb_)])
                if "s" not in SKIP:
                    self.dma(SP, v_d[sg * 512 + tt_ * 128:sg * 512 + (tt_ + 1) * 128, :], vst[:, vb_, :], [("vst", vb_)], [("vd", sg, tt_)])
            if own:
                for h in range(NH):
                    pq, pqk = self.psb("ia", [0, 1])
                    for dk in range(8):
                        self.mm(pq[0:64, :], Win[:, dk, h * HD:(h + 1) * HD], xg[:, xb, dk, :], dk == 0, dk == 7, wink + [("xg", xb)], [pqk])
                    sb_ = cpi % 4
                    self.cp(ACT if cpi % 2 == 0 else DVE, kst[:, sb_, :], pq[0:64, :], [pqk], [("kst", sb_)])
                    cpi += 1
                    if "s" not in SKIP:
                        self.dma(SP, qT_d[h, :, osg * 512:(osg + 1) * 512], kst[:, sb_, :], [("kst", sb_)], [("qTd", osg, h)])
                for c in range(2):
                    pq, pqk = self.psb("ia", [0, 1])
                    for dk in range(8):
                        self.mm(pq[:, :], Win[:, dk, 3 * MIX + c * 128:3 * MIX + (c + 1) * 128], xg[:, xb, dk, :], dk == 0, dk == 7, wink + [("xg", xb)], [pqk])
                    self.cp(ACT, self.qmT[:, c, osg * 512:(osg + 1) * 512], pq[:, :], [pqk], [("XT", 6 + c, osg * 4 + t_) for t_ in range(4)])
        self.cp(DVE, kmb, kmT, ["kmT"], ["kmb"])
        self.dma(POOL, Wo, self.d_wo.rearrange("(k p) f -> p k f", p=128), (), [("WB", 0), ("WB", 1)])
        for b_ in range(0 if "e" in SKIP else 2):
            self.memset(DVE, KA[64:128, b_, 0:2 * T], 0.0, [("KAind", b_), ("WB", 2 + b_)])
            self.dma(POOL, KA[64:80, b_, 0:2 * T], d_eind, (), [("KAind", b_), ("WB", 2 + b_)])

        vd_keys = [("vd", sg, t_) for sg in range(NSG) for t_ in range(4)]
        qbc = 0
        ptc = 0
        for h in range(NH if STAGE >= 2 else 0):
            hp, poff, hb = h // 2, 64 * (h % 2), h % 2
            self.dma(SP, KA[0:64, hb, 0:2 * T], kT_d[h], [("kTd", sg, h) for sg in range(NSG)] + [("WB", 2 + hb)], [("KA", hb)])
            self.dma(SP, QA[0:64, hb, 0:T], qT_d[h], [("qTd", sg, h) for sg in range(NSG // 2)] + [("WB", 5)], [("QA", hb)])
            vb = 0
            if h % 2 == 0:
                self.dma(SP, Vh[:, 0:2 * T // 128, :], v_d[:, hp * 128:(hp + 1) * 128].rearrange("(n p) c -> p n c", p=128), vd_keys, [("Vh", vb), ("WB", 4)])
            kak = [("KA", hb), ("KAind", hb)]
            for i in range(NBo if STAGE >= 3 else 0):
                ob = NBo + i
                qc0 = i * BLK
                bb = qbc % 2
                qbc += 1
                qbk = ("QAb", hb, i)
                for qt in range(2):
                    pgt, pgtk = self.ps[7], ("ps7a", qt)
                    self.mm(pgt[:, qt * 16:(qt + 1) * 16], QA[0:64, hb, qc0 + qt * 128:qc0 + (qt + 1) * 128], kmb[:, h, :], True, True,
                            [("QA", hb), "kmb"], [pgtk])
                    g_ = gm[:, bb, qt, 0:16]
                    gk = ("gm", bb, qt)
                    self.stt(DVE, g_, pgt[:, qt * 16:(qt + 1) * 16], 1.0 / BLK, maskb[:, i, :], ALU.mult, ALU.add, [pgtk, "maskb"], [gk])
                    t8 = gm[:, bb, qt, 16:24]
                    self.S.add(DVE, lambda e, o=t8, a=g_: e.max(out=o, in_=a), [gk], [gk])
                    thr = gm[:, bb, qt, 24:25]
                    self.ts(DVE, thr, t8[:, 2:3], -1.0e29, None, ALU.max, None, [gk], [gk])
                    self.ts(DVE, g_, g_, thr, None, ALU.is_ge, None, [gk], [gk])
                    self.ts(DVE, bqw[:, bb, qt, 64:80], g_, -1.0, -NEG, ALU.add, ALU.mult, [gk], [("bqw", bb)])
                    pB, pBk = self.ps[7], ("ps7b",)
                    self.mm(pB[:, 256 + qt * 128:256 + (qt + 1) * 128], bqw[:, bb, qt, :], self.identb[:, :], True, True,
                            [("bqw", bb), "identb"], [pBk])
                self.cp(ACT, QA[64:128, hb, qc0:qc0 + BLK], self.ps[7][64:128, 256:512], [("ps7b",)], [qbk])
                pso, pok = self.psb("ao", [3, 4])
                psz, pzk = self.psb("az", [5, 6])
                nkt = 2 * (ob + 1)
                if STAGE < 4:
                    continue
                for kt_i in range(nkt):
                    n, kt = kt_i // 2, kt_i % 2
                    kc0 = n * BLK + kt * 128
                    pss, psk = self.psb("as", [0, 1, 2])
                    if n < ob:
                        self.mm(pss[:, 0:BLK], KA[:, hb, kc0:kc0 + 128], QA[:, hb, qc0:qc0 + BLK], True, True,
                                kak + [("QA", hb), qbk], [psk])
                    else:
                        self.mm(pss[:, 0:BLK], KA[0:64, hb, kc0:kc0 + 128], QA[0:64, hb, qc0:qc0 + BLK], True, False,
                                kak + [("QA", hb)], [psk])
                        self.mm(pss[:, 0:BLK], self.identb[:, :], cm[:, kt, :], False, True, ["identb", "cm"], [psk])
                    pb_ = ptc % 3
                    ptc += 1
                    self.act(PT[:, pb_, :], pss[:, 0:BLK], AF.Exp, [psk], [("PT", pb_)], scale=HD ** -0.5)
                    self.mm(pso[:, 0:BLK], Vh[:, n * 2 + kt, :], PT[:, pb_, :], kt_i == 0, kt_i == nkt - 1, [("Vh", vb), ("PT", pb_)], [pok])
                    self.mm(psz[:, 0:BLK], self.onesb[:, :], PT[:, pb_, :], kt_i == 0, kt_i == nkt - 1, ["onesb", ("PT", pb_)], [pzk])
                rb = qbc % 2
                self.S.add(DVE, lambda e, o=rz[poff:poff + 64, rb, :], a=psz[poff:poff + 64, 0:BLK]: e.reciprocal(out=o, in_=a), [pzk], [("rz", rb)])
                self.tt(DVE, XT[poff:poff + 64, hp, qc0:qc0 + BLK], pso[poff:poff + 64, 0:BLK], rz[poff:poff + 64, rb, :], ALU.mult,
                        [pok, ("rz", rb)], [("XT", hp, 2 * i), ("XT", hp, 2 * i + 1)])

        for tg in range(NG):
            self.mem_attention(tg * 512, 512, lambda c, poff, tg=tg: XT[poff:poff + 64, 6 + c, tg * 512:(tg + 1) * 512],
                               lambda c, tg=tg: [("XT", 6 + c, tg * 4 + j) for j in range(4)])
            self.wo_ln1(tg, Wo, ("WB", 0), lambda jk, tt_, tg=tg: XT[:, jk, (tg * 4 + tt_) * 128:(tg * 4 + tt_ + 1) * 128],
                        lambda tt_, tg=tg: [("XT", jk, tg * 4 + tt_) for jk in range(8)] + [("WB", 1)])

    def finish(self):
        NT = self.NT
        self.scr_off = 0
        self.hT = self.carve([128, 2, 4, 512], BF16)
        self.sT = self.carve([128, 2, 512], F32)
        self.xTf = self.carve([128, 2, 8, 128], F32)
        self.load_lnp(1)
        for i in range(NT):
            self.transpose_tile(i, True)
        self.moe()
        for i in range(NT):
            self.ln_tile(i, 1)
        self.dma(SP, self.d_out.rearrange("(n p) d -> p n d", p=128), self.R[:], [("R", i, h) for i in range(NT) for h in range(2)], ["out"])

    def build(self):
        self.setup_common()
        if self.kind == "conv":
            self.conv_mixer()
        else:
            self.moba_mixer()
        self.finish()
        self.S.emit(self.nc)
        self.es.close()
        return self.nc


def _prep_common(i, inp, x_own, memb):
    G = inp["w_rg"].shape[2]
    NE = inp["w_re"].shape[2]
    return {
        "xtok": np.ascontiguousarray(x_own, dtype=np.float32),
        "ident": np.eye(128, dtype=np.float32),
        "memT": np.ascontiguousarray(memb.T),
        "w_mem_kv": inp["w_mem_kv"],
        "w_o": inp["w_o"][i],
        "lnp": np.ascontiguousarray(np.stack([inp["ln1_g"][i], inp["ln1_b"][i], inp["ln2_g"][i], inp["ln2_b"][i]])),
        "w_r": np.ascontiguousarray(np.concatenate([inp["w_rg"][i], inp["w_re"][i]], axis=1)),
        "b_r": np.ascontiguousarray(np.concatenate([inp["b_rg"][i], inp["b_re"][i]])[None, :]),
        "w_gate": inp["w_gate"][i].reshape(NE, D, F),
        "w_up": inp["w_up"][i].reshape(NE, D, F),
        "w_down": inp["w_down"][i].reshape(NE, F, D),
    }


def run_conv_layer(i, j, inp, x, ncores):
    Bt, S, _ = x.shape
    per = ncores // Bt
    T = S // per
    G = inp["w_rg"].shape[2]
    NE = inp["w_re"].shape[2]
    nc = B("conv", T, NE, G).build()
    cwT = inp["conv_dw_w"][j].T.reshape(6, 128, CONVW).transpose(1, 0, 2)
    cv = np.stack([inp["conv_dw_b"][j].reshape(6, 128).T, inp["conv_ln_g"][j].reshape(6, 128).T,
                   inp["conv_ln_b"][j].reshape(6, 128).T], axis=1)
    maps = []
    for c in range(ncores):
        b, h = c // per, c % per
        xo = x[b, h * T:(h + 1) * T]
        halo = x[b, h * T - 32:h * T] if h > 0 else np.zeros((32, D), np.float32)
        m = _prep_common(i, inp, xo, inp["mem"][b])
        m["xTh"] = np.ascontiguousarray(np.concatenate([halo, xo], axis=0).T)
        m["w_in"] = inp["conv_w_in"][j]
        m["conv_w"] = np.ascontiguousarray(cwT)
        m["conv_v"] = np.ascontiguousarray(cv)
        maps.append(m)
    res = run_bass_kernel_spmd(nc, maps, core_ids=list(range(ncores)))
    out = np.empty_like(x)
    for c in range(ncores):
        b, h = c // per, c % per
        out[b, h * T:(h + 1) * T] = res.results[c]["out"]
    return out


def run_moba_layer(i, j, inp, x, ncores):
    Bt, S, _ = x.shape
    per = ncores // Bt
    assert per == 2
    T = S // per
    G = inp["w_rg"].shape[2]
    NE = inp["w_re"].shape[2]
    NBo = T // BLK
    nc = B("moba", T, NE, G).build()
    eind = np.zeros((16, 2 * T), np.float32)
    for jb in range(2 * NBo):
        eind[jb, jb * BLK:(jb + 1) * BLK] = 1.0
    kk = np.arange(128)[:, None]
    qq = np.arange(BLK)[None, :]
    cmask = np.stack([np.where(kk <= qq, 0.0, NEG), np.where(kk + 128 <= qq, 0.0, NEG)], axis=1).astype(np.float32)
    maps = []
    for c in range(ncores):
        b, h = c // per, c % per
        xo = x[b, h * T:(h + 1) * T]
        if h == 0:
            xs = np.concatenate([np.zeros((T, D), np.float32), xo], axis=0)
        else:
            xs = x[b, 0:2 * T]
        padb = np.zeros((1, 16), np.float32)
        padb[0, 2 * NBo:] = -BIG
        if h == 0:
            padb[0, 0:NBo] = -BIG
        m = _prep_common(i, inp, xo, inp["mem"][b])
        m["xsT"] = np.ascontiguousarray(xs.T)
        m["w_in"] = inp["moba_w_in"][j]
        m["padb"] = padb
        m["eind"] = eind
        m["cmask"] = np.ascontiguousarray(cmask)
        maps.append(m)
    res = run_bass_kernel_spmd(nc, maps, core_ids=list(range(ncores)))
    out = np.empty_like(x)
    for c in range(ncores):
        b, h = c // per, c % per
        out[b, h * T:(h + 1) * T] = res.results[c]["out"]
    return out


def kernel(**inp):
    inp = {k: np.asarray(v) for k, v in inp.items()}
    x = inp["x"].astype(np.float32, copy=False)
    x = run_conv_layer(0, 0, inp, x, 8)
    x = run_moba_layer(1, 0, inp, x, 8)
    return x
```
